# Optimizing a Trainium2 kernel written in Bass

```python
import math
import jax, jax.numpy as jnp
from jax import lax
import numpy as np

D_MODEL = 1024
BATCH = 16
SEQ = 2048
DEPTH = 2

CHUNK = 64
Q_BLOCK = 128
NORM_EPS = 1e-6

POOL_WIDTH = D_MODEL // 4
POOL_GROUPS = 4
POOL_GROUP_DIM = POOL_WIDTH // POOL_GROUPS
POOL_WINDOWS = (2, 4, 8, 16)
CONV_WIDTH = D_MODEL // 4
CONV_K = 3
SGU_WIDTH = D_MODEL // 4
SGU_GROUPS = 4
SGU_GROUP_DIM = SGU_WIDTH // SGU_GROUPS
SGU_SEG = 128
DIFF_HEADS = 4
DIFF_QK_DIM = 64
DIFF_V_DIM = 2 * DIFF_QK_DIM
ATTN_WIDTH = DIFF_HEADS * DIFF_V_DIM
REL_BUCKETS = 32
REL_MAX_DIST = 128
N_SOFTMAX_MAPS = 2 * DIFF_HEADS
N_BRANCH = 4
D_FF = 2816
N_EXPERTS = 8
TOP_K = 2
D_FF_EXPERT = 3584
N_DENSE = (DEPTH + 1) // 2
N_MOE = DEPTH // 2

OFF_POOL = 0
OFF_CONV = OFF_POOL + POOL_WIDTH
OFF_SGU = OFF_CONV + 3 * CONV_WIDTH
OFF_ATTN = OFF_SGU + 2 * SGU_WIDTH
OFF_GATE = OFF_ATTN + 3 * ATTN_WIDTH
IN_COLS = OFF_GATE + N_BRANCH * D_MODEL

kernel_name = "hybrid_pool_conv_sgu_diffattn_moe"


def rms_norm(x, g):
    xf = x.astype(jnp.float32)
    y = xf * lax.rsqrt(jnp.mean(xf * xf, axis=-1, keepdims=True) + NORM_EPS)
    return (y * g.astype(jnp.float32)).astype(x.dtype)


def pool_mixer(a, pool_w, pool_scale):
    b_, s_, _ = a.shape
    af = a.astype(jnp.float32).reshape(b_, s_, POOL_GROUPS, POOL_GROUP_DIM)
    cs = jnp.pad(jnp.cumsum(af, axis=1), ((0, 0), (1, 0), (0, 0), (0, 0)))
    pos = jnp.arange(s_)
    outs = []
    for g, w in enumerate(POOL_WINDOWS):
        upper = cs[:, 1:, g]
        lower = jnp.pad(cs[:, :s_ + 1 - w, g], ((0, 0), (w - 1, 0), (0, 0)))
        count = jnp.minimum(pos + 1, w).astype(jnp.float32)[None, :, None]
        outs.append((upper - lower) / count - af[:, :, g])
    pooled = jnp.stack(outs, axis=2).astype(a.dtype)
    mixed = jnp.einsum('bsgc,gcd->bsgd', pooled, pool_w).reshape(b_, s_, POOL_WIDTH)
    return mixed * pool_scale


def short_conv_mixer(b_gate, c_gate, hin, conv_w):
    z = c_gate * hin
    y = lax.conv_general_dilated(
        z, conv_w[:, None, :].astype(z.dtype), window_strides=(1,),
        padding=((CONV_K - 1, 0),), dimension_numbers=('NWC', 'WIO', 'NWC'),
        feature_group_count=CONV_WIDTH)
    return b_gate * y


def spatial_gating_mixer(u, v, ln_g, sgu_w, sgu_b):
    b_, s_, _ = v.shape
    vf = v.astype(jnp.float32)
    mu = jnp.mean(vf, axis=-1, keepdims=True)
    var = jnp.mean(jnp.square(vf - mu), axis=-1, keepdims=True)
    vn = ((vf - mu) * lax.rsqrt(var + NORM_EPS) * ln_g.astype(jnp.float32)).astype(v.dtype)
    vn = vn.reshape(b_, s_ // SGU_SEG, SGU_SEG, SGU_GROUPS, SGU_GROUP_DIM)
    tri = jnp.tril(jnp.ones((SGU_SEG, SGU_SEG), dtype=bool))
    w = jnp.where(tri[None], sgu_w, 0.0)
    s = jnp.einsum('gpq,bnqgc->bnpgc', w, vn) + sgu_b.T[None, None, :, :, None]
    return u * s.reshape(b_, s_, SGU_WIDTH)


def rel_buckets(q_pos, k_pos):
    rel = k_pos[None, :] - q_pos[:, None]
    nb = REL_BUCKETS // 2
    max_exact = nb // 2
    n = jnp.abs(rel)
    nf = jnp.maximum(n, 1).astype(jnp.float32)
    large = max_exact + (jnp.log(nf / max_exact) / math.log(REL_MAX_DIST / max_exact)
                         * (nb - max_exact)).astype(jnp.int32)
    large = jnp.minimum(large, nb - 1)
    return jnp.where(rel > 0, nb, 0) + jnp.where(n < max_exact, n, large)


def diff_attention(q, k, v, rel_bias, q_g, k_g, lam, subln_g, lam_init):
    b_, s_ = q.shape[0], q.shape[1]
    q = rms_norm(q, q_g).transpose(0, 2, 3, 1, 4)
    k = rms_norm(k, k_g).transpose(0, 2, 3, 1, 4)
    v = v.transpose(0, 2, 1, 3)
    scale = DIFF_QK_DIM ** -0.5
    pos = jnp.arange(s_)
    outs = []
    for i in range(s_ // Q_BLOCK):
        q0 = i * Q_BLOCK
        k_end = q0 + Q_BLOCK
        qb = q[:, :, :, q0:k_end]
        kb = k[:, :, :, :k_end]
        vb = v[:, :, :k_end]
        qp = pos[q0:k_end]
        kp = pos[:k_end]
        bias = rel_bias[rel_buckets(qp, kp)].astype(jnp.float32)
        bias = bias.reshape(Q_BLOCK, k_end, DIFF_HEADS, 2).transpose(2, 3, 0, 1)
        mask = (qp[:, None] // CHUNK) >= (kp[None, :] // CHUNK)
        logits = jnp.einsum('bhmqd,bhmkd->bhmqk', qb, kb).astype(jnp.float32) * scale + bias
        logits = jnp.where(mask, logits, -jnp.inf)
        p = jax.nn.softmax(logits, axis=-1)
        a = (p[:, :, 0] - lam * p[:, :, 1]).astype(vb.dtype)
        outs.append(jnp.einsum('bhqk,bhkd->bhqd', a, vb))
    o = jnp.concatenate(outs, axis=2)
    o = rms_norm(o, subln_g) * (1.0 - lam_init)
    return o.transpose(0, 2, 1, 3).reshape(b_, s_, ATTN_WIDTH)


def swiglu(x, w_gate_up, w_down):
    g, u = jnp.split(x @ w_gate_up, 2, axis=-1)
    return (jax.nn.silu(g) * u) @ w_down


def moe_swiglu(xn, router_w, w_gate_up, w_down):
    b_, s_, d_ = xn.shape
    xf = xn.reshape(-1, d_)
    logits = (xf @ router_w).astype(jnp.float32)
    top_v, top_i = lax.top_k(logits, TOP_K)
    top_w = jax.nn.softmax(top_v, axis=-1)
    combine = jnp.sum(jax.nn.one_hot(top_i, N_EXPERTS, dtype=jnp.float32) * top_w[..., None],
                      axis=1).astype(xn.dtype)
    out = jnp.zeros_like(xf)
    for e in range(N_EXPERTS):
        out = out + combine[:, e:e + 1] * swiglu(xf, w_gate_up[e], w_down[e])
    return out.reshape(b_, s_, d_)


def token_mixer(xn, layer, lam_init, rel_bias, w_in, pool_w, pool_scale, conv_w, sgu_ln_g,
                sgu_w, sgu_b, q_norm_g, k_norm_g, diff_lambda, subln_g, w_branch_pool,
                w_branch_conv, w_branch_sgu, w_branch_attn, w_out):
    b_, s_, _ = xn.shape
    z = xn @ w_in[layer]
    y_a = pool_mixer(z[..., OFF_POOL:OFF_CONV], pool_w[layer], pool_scale[layer])
    b_gate, c_gate, hin = jnp.split(z[..., OFF_CONV:OFF_SGU], 3, axis=-1)
    y_b = short_conv_mixer(b_gate, c_gate, hin, conv_w[layer])
    u, v = jnp.split(jax.nn.gelu(z[..., OFF_SGU:OFF_ATTN], approximate=False), 2, axis=-1)
    y_c = spatial_gating_mixer(u, v, sgu_ln_g[layer], sgu_w[layer], sgu_b[layer])
    q, k, va = jnp.split(z[..., OFF_ATTN:OFF_GATE], 3, axis=-1)
    q = q.reshape(b_, s_, DIFF_HEADS, 2, DIFF_QK_DIM)
    k = k.reshape(b_, s_, DIFF_HEADS, 2, DIFF_QK_DIM)
    va = va.reshape(b_, s_, DIFF_HEADS, DIFF_V_DIM)
    lp = diff_lambda[layer].astype(jnp.float32)
    lam = jnp.exp(jnp.sum(lp[0] * lp[1])) - jnp.exp(jnp.sum(lp[2] * lp[3])) + lam_init
    y_d = diff_attention(q, k, va, rel_bias, q_norm_g[layer], k_norm_g[layer], lam,
                         subln_g[layer], lam_init)
    gates = jax.nn.sigmoid(z[..., OFF_GATE:].reshape(b_, s_, N_BRANCH, D_MODEL))
    merged = (gates[:, :, 0] * (y_a @ w_branch_pool[layer])
              + gates[:, :, 1] * (y_b @ w_branch_conv[layer])
              + gates[:, :, 2] * (y_c @ w_branch_sgu[layer])
              + gates[:, :, 3] * (y_d @ w_branch_attn[layer]))
    return merged @ w_out[layer]


def setup_inputs(seed: int = 0) -> dict:
    key = jax.random.key(seed)
    ks = jax.random.split(key, 32)
    f32 = jnp.float32
    L = DEPTH

    def nrm(k, shape, scale):
        return jax.random.normal(k, shape, f32) * scale

    def gain(k, shape):
        return 1.0 + 0.02 * jax.random.normal(k, shape, f32)

    return {
        "x": jax.random.normal(ks[0], (BATCH, SEQ, D_MODEL), f32),
        "rel_bias": nrm(ks[1], (REL_BUCKETS, N_SOFTMAX_MAPS), 0.5),
        "norm1_g": gain(ks[2], (L, D_MODEL)),
        "w_in": nrm(ks[3], (L, D_MODEL, IN_COLS), D_MODEL ** -0.5),
        "pool_w": nrm(ks[4], (L, POOL_GROUPS, POOL_GROUP_DIM, POOL_GROUP_DIM), POOL_GROUP_DIM ** -0.5),
        "pool_scale": 1.0 + 0.1 * jax.random.normal(ks[5], (L, POOL_WIDTH), f32),
        "conv_w": nrm(ks[6], (L, CONV_K, CONV_WIDTH), CONV_K ** -0.5),
        "sgu_ln_g": gain(ks[7], (L, SGU_WIDTH)),
        "sgu_w": nrm(ks[8], (L, SGU_GROUPS, SGU_SEG, SGU_SEG), SGU_SEG ** -0.5),
        "sgu_b": 1.0 + 0.1 * jax.random.normal(ks[9], (L, SGU_GROUPS, SGU_SEG), f32),
        "q_norm_g": gain(ks[10], (L, DIFF_QK_DIM)),
        "k_norm_g": gain(ks[11], (L, DIFF_QK_DIM)),
        "diff_lambda": nrm(ks[12], (L, 4, DIFF_QK_DIM), 0.1),
        "subln_g": gain(ks[13], (L, DIFF_V_DIM)),
        "w_branch_pool": nrm(ks[14], (L, POOL_WIDTH, D_MODEL), POOL_WIDTH ** -0.5),
        "w_branch_conv": nrm(ks[15], (L, CONV_WIDTH, D_MODEL), CONV_WIDTH ** -0.5),
        "w_branch_sgu": nrm(ks[16], (L, SGU_WIDTH, D_MODEL), SGU_WIDTH ** -0.5),
        "w_branch_attn": nrm(ks[17], (L, ATTN_WIDTH, D_MODEL), ATTN_WIDTH ** -0.5),
        "w_out": nrm(ks[18], (L, D_MODEL, D_MODEL), D_MODEL ** -0.5),
        "norm2_g": gain(ks[19], (L, D_MODEL)),
        "ffn_w_gate_up": nrm(ks[20], (N_DENSE, D_MODEL, 2 * D_FF), D_MODEL ** -0.5),
        "ffn_w_down": nrm(ks[21], (N_DENSE, D_FF, D_MODEL), D_FF ** -0.5),
        "router_w": nrm(ks[22], (N_MOE, D_MODEL, N_EXPERTS), D_MODEL ** -0.5),
        "moe_w_gate_up": nrm(ks[23], (N_MOE, N_EXPERTS, D_MODEL, 2 * D_FF_EXPERT), D_MODEL ** -0.5),
        "moe_w_down": nrm(ks[24], (N_MOE, N_EXPERTS, D_FF_EXPERT, D_MODEL), D_FF_EXPERT ** -0.5),
    }


def reference(x, rel_bias, norm1_g, w_in, pool_w, pool_scale, conv_w, sgu_ln_g, sgu_w, sgu_b,
              q_norm_g, k_norm_g, diff_lambda, subln_g, w_branch_pool, w_branch_conv,
              w_branch_sgu, w_branch_attn, w_out, norm2_g, ffn_w_gate_up, ffn_w_down,
              router_w, moe_w_gate_up, moe_w_down):
    h = x
    for layer in range(DEPTH):
        lam_init = 0.8 - 0.6 * math.exp(-0.3 * layer)
        xn = rms_norm(h, norm1_g[layer])
        h = h + token_mixer(xn, layer, lam_init, rel_bias, w_in, pool_w, pool_scale, conv_w,
                            sgu_ln_g, sgu_w, sgu_b, q_norm_g, k_norm_g, diff_lambda, subln_g,
                            w_branch_pool, w_branch_conv, w_branch_sgu, w_branch_attn, w_out)
        hn = rms_norm(h, norm2_g[layer])
        if layer % 2 == 0:
            h = h + swiglu(hn, ffn_w_gate_up[layer // 2], ffn_w_down[layer // 2])
        else:
            h = h + moe_swiglu(hn, router_w[layer // 2], moe_w_gate_up[layer // 2],
                               moe_w_down[layer // 2])
    return h
```

```python
import math
from contextlib import ExitStack

import numpy as np
import concourse.bass as bass
import concourse.mybir as mybir
from concourse.bass_utils import run_bass_kernel_spmd

F32 = mybir.dt.float32
BF16 = mybir.dt.bfloat16
AF = mybir.ActivationFunctionType
ALU = mybir.AluOpType
AX = mybir.AxisListType

N_CORES = 8
D = 1024
SEQ = 2048
HALF = 1024
IN_COLS = 7168
OFF_CONV, OFF_SGU, OFF_ATTN, OFF_GATE = 256, 1024, 1536, 3072
D_FF = 2816
D_FFE = 3584
NE = 8
EPS = 1e-6
SLOT = 12288
NEG = -30000.0
NPV = 16


class Buf:
    __slots__ = ("name", "w", "r")

    def __init__(self, name):
        self.name = name
        self.w = None
        self.r = {}


class Sched:
    def __init__(self, nc, stack):
        self.nc = nc
        self.E = {"pe": nc.tensor, "act": nc.scalar, "dve": nc.vector, "pool": nc.gpsimd, "sp": nc.sync}
        self.sems = {}
        self.cnt = {}
        self.seen = {k: {} for k in self.E}
        self.stack = stack
        for k in ("pe", "act", "dve", "pool"):
            self.sems[k] = stack.enter_context(nc.semaphore("prog_" + k))
            self.cnt[k] = 0
        self.n_instr = 0
        self.n_wait = 0

    def new_dma_sem(self, name):
        self.sems[name] = self.stack.enter_context(self.nc.semaphore(name))
        self.cnt[name] = 0
        return name

    def make_pools(self, n_sp=40, n_pool=8):
        self.pools = {"sp": [self.new_dma_sem(f"dsp{i}") for i in range(n_sp)],
                      "pool": [self.new_dma_sem(f"dpl{i}") for i in range(n_pool)]}
        self.pool_rr = {"sp": 0, "pool": 0}

    def _need(self, e, dep):
        if dep is None:
            return
        key, val = dep
        if self.seen[e].get(key, 0) >= val:
            return
        self.E[e].wait_ge(self.sems[key], val)
        self.seen[e][key] = val
        self.n_wait += 1

    def deps(self, e, reads, writes):
        for b in reads:
            self._need(e, b.w)
        for b in writes:
            self._need(e, b.w)
            for key, val in b.r.items():
                self._need(e, (key, val))

    def done(self, key, val, reads, writes):
        for b in reads:
            if b.r.get(key, 0) < val:
                b.r[key] = val
        for b in writes:
            b.w = (key, val)
            b.r = {}

    def op(self, e, fn, reads=(), writes=()):
        self.deps(e, reads, writes)
        ins = fn()
        self.cnt[e] += 1
        ins.then_inc(self.sems[e], 1)
        self.done(e, self.cnt[e], reads, writes)
        self.n_instr += 1
        return ins

    def mm(self, out_ap, pairs, out_buf, reads, start=True, stop=True):
        self.deps("pe", reads, [out_buf])
        n = len(pairs)
        ins = None
        for i, (l, r) in enumerate(pairs):
            ins = self.nc.tensor.matmul(out_ap, l, r, start=(start and i == 0), stop=(stop and i == n - 1))
        self.n_instr += n
        self.cnt["pe"] += 1
        ins.then_inc(self.sems["pe"], 1)
        self.done("pe", self.cnt["pe"], reads, [out_buf])
        return ins

    def pe_multi(self, fns, out_buf, reads):
        self.deps("pe", reads, [out_buf])
        ins = None
        for f in fns:
            ins = f()
        self.n_instr += len(fns)
        self.cnt["pe"] += 1
        ins.then_inc(self.sems["pe"], 1)
        self.done("pe", self.cnt["pe"], reads, [out_buf])
        return ins

    def dma(self, q, out_ap, in_ap, sem, reads=(), writes=(), **kw):
        if sem is None:
            sem = self.pools[q][self.pool_rr[q] % len(self.pools[q])]
            self.pool_rr[q] += 1
            if self.cnt[sem] > 0:
                self._need(q, (sem, self.cnt[sem]))
        self.deps(q, reads, writes)
        ins = self.E[q].dma_start(out=out_ap, in_=in_ap, **kw)
        self.cnt[sem] += 16
        ins.then_inc(self.sems[sem], 16)
        self.done(sem, self.cnt[sem], reads, writes)
        self.n_instr += 1
        return ins


class Prog:
    def __init__(self, cfg=None):
        self.cfg = dict(n_seq=2, n_layers=2, ffn=True, stop_after=None, experts=NE)
        if cfg:
            self.cfg.update(cfg)
        self.uid = 0

    def sb(self, st, name, shape, dtype):
        self.uid += 1
        t = st.enter_context(self.nc.sbuf_tensor(f"{name}_{self.uid}", shape, dtype))
        return t, Buf(name)

    def bank(self, pool=None):
        pool = pool or self.rr_all
        i = pool[self.rr_i % len(pool)]
        self.rr_i += 1
        return self.ps[i], self.psb[i]

    def wacquire(self, tag):
        k = self.w_k
        assert self.w_plan[k][0] == tag, (k, tag, self.w_plan[k][0])
        self.wissue(k + 2)
        self.w_k += 1
        s = k % 2
        return self.wslot[s], self.wbuf[s]

    def wissue(self, upto):
        S = self.S
        while self.w_issued < min(upto, len(self.w_plan)):
            k = self.w_issued
            s = k % 2
            if S.cnt[self.wsem[s]] > 0:
                S._need("pool", (self.wsem[s], S.cnt[self.wsem[s]]))
            for (off, dims, src) in self.w_plan[k][1]:
                n = int(np.prod(dims))
                dst = self.wslot[s][:, off:off + n]
                if len(dims) == 2:
                    dst = dst.rearrange("p (a b) -> p a b", a=dims[0])
                elif len(dims) == 3:
                    dst = dst.rearrange("p (a b c) -> p a b c", a=dims[0], b=dims[1])
                S.dma("pool", dst, src, self.wsem[s], writes=[self.wbuf[s]])
            self.w_issued += 1

    def make_plan(self):
        c = self.cfg
        plan = []
        I = self.I
        for _seq in range(c["n_seq"]):
            for l in range(c["n_layers"]):
                Win = I["w_in"][l].rearrange("(kc p) n -> p kc n", p=128)
                for _hf in range(2):
                    plan.append(("qkv", [(0, (8, 1024), Win[:, :, OFF_ATTN:OFF_ATTN + 1024]),
                                         (8192, (8, 512), Win[:, :, OFF_ATTN + 1024:OFF_GATE])]))
                    plan.append(("p3", [(0, (8, 1536), Win[:, :, 0:OFF_ATTN])]))
                    Wg = Win[:, :, OFF_GATE:IN_COLS].rearrange("p k (i n) -> p k i n", i=4)
                    for cp in range(4):
                        ent = [(i * 2048, (8, 256), Wg[:, :, i, cp * 256:(cp + 1) * 256]) for i in range(4)]
                        off = 8192
                        for nm, nk in (("w_branch_pool", 2), ("w_branch_conv", 2), ("w_branch_sgu", 2), ("w_branch_attn", 4)):
                            Wb = I[nm][l].rearrange("(kc p) n -> p kc n", p=128)
                            ent.append((off, (nk, 256), Wb[:, :, cp * 256:(cp + 1) * 256]))
                            off += nk * 256
                        plan.append(("p4", ent))
                    plan.append(("wout", [(0, (8, 1024), I["w_out"][l].rearrange("(kc p) n -> p kc n", p=128))]))
                if not c["ffn"]:
                    continue
                for (e, j0, nch) in self.ffn_groups(l):
                    if e is None:
                        Wgu = I["ffn_w_gate_up"][0].rearrange("(kc p) n -> p kc n", p=128)
                        Wd = I["ffn_w_down"][0].rearrange("(c p) n -> p c n", p=128)
                        dff = D_FF
                    else:
                        Wgu = I["moe_w_gate_up"][0, e].rearrange("(kc p) n -> p kc n", p=128)
                        Wd = I["moe_w_down"][0, e].rearrange("(c p) n -> p c n", p=128)
                        dff = D_FFE
                    nc_ = nch * 128
                    plan.append(("ffn", [(0, (8, nc_), Wgu[:, :, j0 * 128:j0 * 128 + nc_]),
                                         (4096, (8, nc_), Wgu[:, :, dff + j0 * 128:dff + j0 * 128 + nc_]),
                                         (8192, (nch, 1024), Wd[:, j0:j0 + nch, :])]))
        return plan

    def ffn_groups(self, l):
        if l % 2 == 0:
            return [(None, j0, min(4, 22 - j0)) for j0 in range(0, 22, 4)]
        return [(e, j0, 4) for e in range(self.cfg["experts"]) for j0 in range(0, 28, 4)]

    def build(self):
        nc = bass.Bass("TRN2", target_bir_lowering=False)
        self.nc = nc
        I = {}

        def din(name, shape):
            I[name] = nc.dram_tensor(name, list(shape), F32, kind="ExternalInput").ap()

        din("x", (2, SEQ, D))
        din("w_in", (2, D, IN_COLS))
        din("w_branch_pool", (2, 256, D)); din("w_branch_conv", (2, 256, D)); din("w_branch_sgu", (2, 256, D))
        din("w_branch_attn", (2, 512, D)); din("w_out", (2, D, D))
        din("ffn_w_gate_up", (1, D, 2 * D_FF)); din("ffn_w_down", (1, D_FF, D))
        din("router_w", (1, D, NE)); din("moe_w_gate_up", (1, NE, D, 2 * D_FFE)); din("moe_w_down", (1, NE, D_FFE, D))
        din("norm1_g", (2, D)); din("norm2_g", (2, D)); din("sgu_ln_g", (2, 256)); din("diff_lambda", (2, 256))
        din("pvec", (2, 128, NPV)); din("bb", (2, 128, 2, 512)); din("wst", (2, 128, 4, 128)); din("pwbd", (2, 128, 2, 128))
        din("strips", (128, 8, 640)); din("c15", (128, 8))
        din("ident", (128, 128)); din("trimask", (128, 128)); din("corr", (128, 2, 16)); din("blockones", (128, 128))
        out = nc.dram_tensor("out", [2, SEQ, D], F32, kind="ExternalOutput").ap()
        scr = nc.dram_tensor("kv_scr", [128, 8192], BF16, kind="Internal").ap()
        self.I = I
        self.scr = scr
        self.scr_buf = Buf("scr")

        with ExitStack() as st:
            S = Sched(nc, st)
            self.S = S
            self.ps, self.psb = [], []
            for i in range(8):
                self.ps.append(st.enter_context(nc.psum_tensor(f"psb{i}", [128, 512], F32)))
                self.psb.append(Buf(f"ps{i}"))
            self.rr_all = list(range(8))
            self.rr_i = 0
            self.wslot, self.wbuf, self.wsem = [], [], []
            for i in range(2):
                t, b = self.sb(st, f"wslot{i}", [128, SLOT], BF16)
                self.wslot.append(t); self.wbuf.append(b); self.wsem.append(S.new_dma_sem(f"wsem{i}"))
            self.w_plan = self.make_plan()
            self.w_k = 0
            self.w_issued = 0
            S.make_pools()

            self.h, _hb = self.sb(st, "h", [128, 16, D], F32)
            self.hq = [Buf(f"hq{i}") for i in range(4)]
            self.ident, self.identb = self.sb(st, "ident", [128, 128], BF16)
            self.ones, self.onesb = self.sb(st, "ones", [128, 128], BF16)
            self.bones, self.bonesb = self.sb(st, "bones", [128, 128], BF16)
            self.eps, self.epsb = self.sb(st, "eps", [128, 1], F32)
            self.c15, self.c15b = self.sb(st, "c15", [128, 8], F32)
            self.corr, self.corrb = self.sb(st, "corr", [128, 2, 16], F32)
            S.dma("pool", self.ident[:], I["ident"][:, :], None, writes=[self.identb])
            S.dma("pool", self.bones[:], I["blockones"][:, :], None, writes=[self.bonesb])
            S.dma("sp", self.c15[:], I["c15"][:, :], None, writes=[self.c15b])
            S.dma("sp", self.corr[:], I["corr"][:, :, :], None, writes=[self.corrb])
            S.op("dve", lambda: nc.vector.memset(self.ones[:], 1.0), writes=[self.onesb])
            S.op("dve", lambda: nc.vector.memset(self.eps[:], EPS), writes=[self.epsb])
            self.pv, self.pvb = self.sb(st, "pv", [128, NPV], F32)
            self.gbc1, self.gbc1b = self.sb(st, "gbc1", [128, D], F32)
            self.lngbc, self.lngbcb = self.sb(st, "lngbc", [128, 256], F32)
            self.wst, self.wstb = self.sb(st, "wst", [128, 4, 128], BF16)
            self.pwbd, self.pwbdb = self.sb(st, "pwbd", [128, 2, 128], BF16)
            self.lv, self.lvb = self.sb(st, "lv", [128, 8], F32)
            self.hist_a, self.hist_ab = self.sb(st, "hist_a", [128, 2, 15], F32)
            self.hist_z, self.hist_zb = self.sb(st, "hist_z", [128, 2, 2], F32)

            for seq in range(self.cfg["n_seq"]):
                xv = I["x"][seq].rearrange("(tb p) d -> p tb d", p=128)
                for q4 in range(4):
                    S.dma("sp", self.h[:, q4 * 4:(q4 + 1) * 4, :], xv[:, q4 * 4:(q4 + 1) * 4, :], None, writes=[self.hq[q4]])
                ov = out[seq].rearrange("(tb p) d -> p tb d", p=128)
                stored = set()

                def store_q(q4, ov=ov, stored=stored):
                    if q4 in stored:
                        return
                    stored.add(q4)
                    S.dma("sp", ov[:, q4 * 4:(q4 + 1) * 4, :], self.h[:, q4 * 4:(q4 + 1) * 4, :], None, reads=[self.hq[q4]])

                for l in range(self.cfg["n_layers"]):
                    self.layer_setup(st, l)
                    for hf in range(2):
                        self.token_mixer(l, hf)
                    self.store_cb = store_q if (l == self.cfg["n_layers"] - 1) else None
                    if self.cfg["ffn"]:
                        self.ffn_phase(l)
                for q4 in range(4):
                    store_q(q4)
            for nm in S.pools["sp"] + S.pools["pool"] + self.wsem:
                if S.cnt[nm] > 0:
                    S._need("sp", (nm, S.cnt[nm]))
            assert self.w_k == len(self.w_plan), (self.w_k, len(self.w_plan))
            self.stats = (S.n_instr, S.n_wait)
        return nc

    def layer_setup(self, st, l):
        nc, S, I = self.nc, self.S, self.I
        lam_init = 0.8 - 0.6 * math.exp(-0.3 * l)
        S.dma("sp", self.pv[:], I["pvec"][l], None, writes=[self.pvb])
        S.dma("sp", self.gbc1[:], I["norm1_g"][l].partition_broadcast(128), None, writes=[self.gbc1b])
        S.dma("sp", self.lngbc[:], I["sgu_ln_g"][l].partition_broadcast(128), None, writes=[self.lngbcb])
        S.dma("pool", self.pwbd[:], I["pwbd"][l], None, writes=[self.pwbdb])
        with ExitStack() as ls:
            wf, wfb = self.sb(ls, "wstf", [128, 4, 128], F32)
            tm, tmb = self.sb(ls, "trim", [128, 128], F32)
            pr, prb = self.sb(ls, "lamprod", [128, 128], F32)
            self.dl, self.dlb = self.sb(ls, "dl", [128, 256], F32)
            self.fresh([wfb, tmb, prb, self.dlb])
            S.dma("sp", self.dl[:], I["diff_lambda"][l].partition_broadcast(128), None, writes=[self.dlb])
            S.dma("sp", wf[:], I["wst"][l], None, writes=[wfb])
            S.dma("sp", tm[:], I["trimask"][:, :], None, writes=[tmb])
            for g in range(4):
                S.op("dve", lambda: nc.vector.tensor_tensor(out=self.wst[:, g, :], in0=wf[:, g, :], in1=tm[:], op=ALU.mult),
                     reads=[wfb, tmb], writes=[self.wstb])
            dl3 = self.dl[:].rearrange("p (a b) -> p a b", a=4)
            pr3 = pr[:].rearrange("p (a b) -> p a b", a=2)
            S.op("dve", lambda: nc.vector.tensor_tensor(out=pr3[:, 0, :], in0=dl3[:, 0, :], in1=dl3[:, 1, :], op=ALU.mult),
                 reads=[self.dlb], writes=[prb])
            S.op("dve", lambda: nc.vector.tensor_tensor(out=pr3[:, 1, :], in0=dl3[:, 2, :], in1=dl3[:, 3, :], op=ALU.mult),
                 reads=[self.dlb], writes=[prb])
            S.op("dve", lambda: nc.vector.reduce_sum(out=self.lv[:, 4:6], in_=pr3, axis=AX.X), reads=[prb], writes=[self.lvb])
            S.op("act", lambda: nc.scalar.activation(out=self.lv[:, 6:8], in_=self.lv[:, 4:6], func=AF.Exp),
                 reads=[self.lvb], writes=[self.lvb])
            S.op("dve", lambda: nc.vector.tensor_tensor(out=self.lv[:, 0:1], in0=self.lv[:, 6:7], in1=self.lv[:, 7:8], op=ALU.subtract),
                 reads=[self.lvb], writes=[self.lvb])
            S.op("dve", lambda: nc.vector.tensor_scalar(out=self.lv[:, 0:1], in0=self.lv[:, 0:1], scalar1=lam_init, scalar2=None, op0=ALU.add),
                 reads=[self.lvb], writes=[self.lvb])
            S.op("dve", lambda: nc.vector.tensor_scalar(out=self.lv[:, 1:2], in0=self.pv[:, 8:9], scalar1=0.125, scalar2=None, op0=ALU.mult),
                 reads=[self.pvb, self.lvb], writes=[self.lvb])
            S.op("dve", lambda: nc.vector.tensor_copy(out=self.lv[:, 2:3], in_=self.pv[:, 9:10]), reads=[self.pvb, self.lvb], writes=[self.lvb])
            S.op("dve", lambda: nc.vector.tensor_scalar(out=self.lv[:, 3:4], in0=self.pv[:, 10:11], scalar1=1.0 - lam_init, scalar2=None, op0=ALU.mult),
                 reads=[self.pvb, self.lvb], writes=[self.lvb])
            self.fence([wfb, tmb, prb, self.dlb])

    def fence(self, bufs):
        for b in bufs:
            self.released.append(b)

    def norm_phase(self, st, tb0, ntb, gbc, gbcb, dstT, dstTb, router=None, after_block=None):
        nc, S = self.nc, self.S
        ss, ssb = self.sb(st, "ss", [128, 16], F32)
        rs, rsb = self.sb(st, "rs", [128, 16], F32)
        junk, junkb = self.sb(st, "junk", [128, D], BF16)
        xs = [self.sb(st, f"xs{i}", [128, D], BF16) for i in range(2)]
        self.fresh([ssb, rsb, junkb, xs[0][1], xs[1][1]])
        for i in range(ntb):
            S.op("act", lambda: nc.scalar.activation(out=junk[:], in_=self.h[:, tb0 + i, :], func=AF.Square, accum_out=ss[:, i:i + 1]),
                 reads=[self.hq[(tb0 + i) // 4]], writes=[junkb, ssb])
        S.op("act", lambda: nc.scalar.activation(out=rs[:, 0:ntb], in_=ss[:, 0:ntb], func=AF.Ln, bias=self.eps[:], scale=1.0 / D),
             reads=[ssb, self.epsb], writes=[rsb])
        S.op("act", lambda: nc.scalar.activation(out=rs[:, 0:ntb], in_=rs[:, 0:ntb], func=AF.Exp, scale=-0.5), reads=[rsb], writes=[rsb])
        for i in range(ntb):
            x_t, x_b = xs[i % 2]
            S.op("dve", lambda: nc.vector.scalar_tensor_tensor(out=x_t[:], in0=self.h[:, tb0 + i, :], scalar=rs[:, i:i + 1], in1=gbc[:],
                                                               op0=ALU.mult, op1=ALU.mult), reads=[self.hq[(tb0 + i) // 4], rsb, gbcb], writes=[x_b])
            pt, pb = self.bank()
            ptv = pt[:].bitcast(BF16)
            S.pe_multi([(lambda c=c: nc.tensor.transpose(out=ptv[:, c * 128:(c + 1) * 128], in_=x_t[:, c * 128:(c + 1) * 128], identity=self.ident[:]))
                        for c in range(8)], pb, [x_b, self.identb])
            eng = "act" if i % 2 == 0 else "dve"
            dst = dstT[:, :, i * 128:(i + 1) * 128]
            src = ptv.rearrange("p (c t) -> p c t", c=8)
            if eng == "act":
                S.op("act", lambda: nc.scalar.copy(out=dst, in_=src), reads=[pb], writes=[dstTb])
            else:
                S.op("dve", lambda: nc.vector.tensor_copy(out=dst, in_=src), reads=[pb], writes=[dstTb])
            if router is not None:
                router(i, x_t, x_b, rs, rsb)
            if after_block is not None:
                after_block(i)
        self.fence([ssb, rsb, junkb, xs[0][1], xs[1][1]])

    def fresh(self, bufs):
        if not self.released:
            return
        merged_w = []
        merged_r = {}
        for b in self.released:
            if b.w is not None:
                merged_w.append(b.w)
            for k, v in b.r.items():
                merged_r[k] = max(merged_r.get(k, 0), v)
        for k, v in merged_w:
            merged_r[k] = max(merged_r.get(k, 0), v)
        for b in bufs:
            for k, v in merged_r.items():
                b.r[k] = max(b.r.get(k, 0), v)
        z = Buf("released")
        z.r = merged_r
        self.released = [z]

    def token_mixer(self, l, hf):
        nc, S, I = self.nc, self.S, self.I
        with ExitStack() as st:
            xnT, xnTb = self.sb(st, "xnT", [128, 8, HALF], BF16)
            yd, ydb = self.sb(st, "yd", [128, 4, HALF], BF16)
            self.fresh([xnTb, ydb])
            with ExitStack() as s1:
                self.norm_phase(s1, hf * 8, 8, self.gbc1, self.gbc1b, xnT, xnTb)
            with ExitStack() as s2:
                self.attention(s2, l, hf, xnT, xnTb, yd, ydb)
            yabc, yabcb = self.sb(st, "yabc", [128, 6, HALF], BF16)
            self.fresh([yabcb])
            wt, wb = self.wacquire("p3")
            with ExitStack() as s3:
                self.p3_sgu(s3, l, hf, xnT, xnTb, yabc, yabcb, wt, wb)
            with ExitStack() as s3:
                self.p3_poolconv(s3, l, hf, xnT, xnTb, yabc, yabcb, wt, wb)
            with ExitStack() as s4:
                self.p45(s4, l, hf, xnT, xnTb, yabc, yabcb, yd, ydb)
            self.fence([xnTb, yabcb, ydb])

    def attention(self, st, l, hf, xnT, xnTb, yd, ydb):
        nc, S, I = self.nc, self.S, self.I
        t0 = hf * HALF
        kT, kTb = self.sb(st, "kT", [128, 4, SEQ], BF16)
        V, Vb = self.sb(st, "V", [128, 16, 512], BF16)
        qh = [self.sb(st, f"qh{i}", [128, HALF], BF16) for i in range(4)]
        strips, stripsb = self.sb(st, "strips", [128, 8, 640], BF16)
        sq = [self.sb(st, f"sq{i}", [128, 512], BF16) for i in range(2)]
        f32t = [self.sb(st, f"at{i}", [128, 512], F32) for i in range(4)]
        Et = [self.sb(st, f"E{i}", [128, 512], BF16) for i in range(4)]
        allb = [kTb, Vb, stripsb] + [b for _, b in qh] + [b for _, b in sq] + [b for _, b in f32t] + [b for _, b in Et]
        self.fresh(allb)
        S.dma("pool", strips[:], I["strips"][:, :, :], None, writes=[stripsb])
        if hf == 1:
            S.dma("sp", kT[:, :, 0:HALF], self.scr[:, 0:4096].rearrange("p (a b) -> p a b", a=4), None, reads=[self.scr_buf], writes=[kTb])
            S.dma("sp", V[:, 0:8, :], self.scr[:, 4096:8192].rearrange("p (a b) -> p a b", a=8), None, reads=[self.scr_buf], writes=[Vb])
        wt, wb = self.wacquire("qkv")
        Wqk = wt[:, 0:8192].rearrange("p (k n) -> p k n", k=8)
        Wv = wt[:, 8192:12288].rearrange("p (k n) -> p k n", k=8)
        self.qk_cnt = 0

        def proj(which, hd, tt, dst, dstb, g, qz=None):
            bp = None if qz is None else [0, 1, 2, 3]
            A, Ab = self.bank(bp)
            c0 = which * 512 + hd * 128
            S.mm(A[:], [(Wqk[:, kc, c0:c0 + 128], xnT[:, kc, tt * 512:(tt + 1) * 512]) for kc in range(8)], Ab, [wb, xnTb])
            sq_t, sq_b = sq[self.qk_cnt % 2]
            rs_t, rs_b = f32t[self.qk_cnt % 2]
            self.qk_cnt += 1
            S.op("act", lambda: nc.scalar.activation(out=sq_t[:], in_=A[:], func=AF.Square), reads=[Ab], writes=[sq_b])
            B, Bb = self.bank(bp)
            S.mm(B[:], [(self.bones[:], sq_t[:])], Bb, [self.bonesb, sq_b])
            S.op("act", lambda: nc.scalar.activation(out=rs_t[:], in_=B[:], func=AF.Ln, bias=self.eps[:], scale=1.0 / 64),
                 reads=[Bb, self.epsb], writes=[rs_b])
            S.op("act", lambda: nc.scalar.activation(out=rs_t[:], in_=rs_t[:], func=AF.Exp, scale=-0.5), reads=[rs_b], writes=[rs_b])
            if qz is None:
                S.op("dve", lambda: nc.vector.scalar_tensor_tensor(out=dst, in0=A[:], scalar=g, in1=rs_t[:], op0=ALU.mult, op1=ALU.mult),
                     reads=[Ab, rs_b, self.lvb], writes=[dstb])
            else:
                for m in range(2):
                    pr = slice(m * 64, (m + 1) * 64)
                    S.op("dve", lambda: nc.vector.scalar_tensor_tensor(out=qz[m][0][pr, tt * 512:(tt + 1) * 512], in0=A[pr, :], scalar=g[pr, :], in1=rs_t[pr, :],
                                                                       op0=ALU.mult, op1=ALU.mult), reads=[Ab, rs_b, self.lvb], writes=[qz[m][1]])

        for i4 in range(4):
            m = i4 % 2
            zr = slice((1 - m) * 64, (2 - m) * 64)
            S.op("dve", lambda: nc.vector.memset(qh[i4][0][zr, :], 0.0), writes=[qh[i4][1]])
        for hd in range(4):
            for tt in range(2):
                proj(1, hd, tt, kT[:, hd, t0 + tt * 512:t0 + (tt + 1) * 512], kTb, self.lv[:, 2:3])
        for tb in range(8):
            C, Cb = self.bank()
            S.mm(C[:], [(xnT[:, kc, tb * 128:(tb + 1) * 128], Wv[:, kc, :]) for kc in range(8)], Cb, [wb, xnTb])
            if tb % 2 == 0:
                S.op("act", lambda: nc.scalar.copy(out=V[:, hf * 8 + tb, :], in_=C[:]), reads=[Cb], writes=[Vb])
            else:
                S.op("dve", lambda: nc.vector.tensor_copy(out=V[:, hf * 8 + tb, :], in_=C[:]), reads=[Cb], writes=[Vb])
        if hf == 0:
            S.dma("sp", self.scr[:, 0:4096].rearrange("p (a b) -> p a b", a=4), kT[:, :, 0:HALF], None, reads=[kTb], writes=[self.scr_buf])
            S.dma("sp", self.scr[:, 4096:8192].rearrange("p (a b) -> p a b", a=8), V[:, 0:8, :], None, reads=[Vb], writes=[self.scr_buf])
        spool = [0, 1, 2, 3]
        O = [(self.ps[4 + m], self.psb[4 + m]) for m in range(2)]
        L = [(self.ps[6 + m], self.psb[6 + m]) for m in range(2)]
        items = [(hd, qt, kb) for hd in range(4) for qt in range(2) for kb in range(4 * (hf * 2 + qt) + 4)]
        state = {}

        def stage_a(idx):
            hd, qt, kb = items[idx]
            qz = [qh[(hd % 2) * 2 + m] for m in range(2)]
            if qt == 0 and kb == 0:
                for tt in range(2):
                    proj(0, hd, tt, None, None, self.lv[:, 1:2], qz=qz)
            gq = hf * 2 + qt
            j = kb - 4 * gq
            c0 = max(0, j) * 128
            near = j >= -1
            q0 = qt * 512
            for m in range(2):
                mp = hd * 2 + m
                bi = spool[(2 * idx + m) % 4]
                Sb_t, Sb_b = self.ps[bi], self.psb[bi]
                pairs = [(kT[:, hd, kb * 128:(kb + 1) * 128], qz[m][0][:, q0 + c0:q0 + 512])]
                rd = [kTb, qz[m][1]]
                if near:
                    s0 = c0 - j * 128
                    pairs.append((self.ident[:], strips[:, mp, s0:s0 + 512 - c0]))
                    rd += [self.identb, stripsb]
                S.mm(Sb_t[:, c0:512], pairs, Sb_b, rd)
                E_t, E_b = Et[(2 * idx + m) % 4]
                if near:
                    S.op("act", lambda: nc.scalar.activation(out=E_t[:, c0:512], in_=Sb_t[:, c0:512], func=AF.Exp), reads=[Sb_b], writes=[E_b])
                else:
                    S.op("act", lambda: nc.scalar.activation(out=E_t[:, c0:512], in_=Sb_t[:, c0:512], func=AF.Exp, bias=self.c15[:, mp:mp + 1]),
                         reads=[Sb_b, self.c15b], writes=[E_b])

        def stage_b(idx):
            hd, qt, kb = items[idx]
            gq = hf * 2 + qt
            nkb = 4 * gq + 4
            c0 = max(0, kb - 4 * gq) * 128
            for m in range(2):
                E_t, E_b = Et[(2 * idx + m) % 4]
                S.mm(O[m][0][:, c0:512], [(V[:, kb, hd * 128:(hd + 1) * 128], E_t[:, c0:512])], O[m][1], [Vb, E_b],
                     start=(kb == 0), stop=(kb == nkb - 1))
                S.mm(L[m][0][:, c0:512], [(self.ones[:], E_t[:, c0:512])], L[m][1], [self.onesb, E_b],
                     start=(kb == 0), stop=(kb == nkb - 1))
            if kb == nkb - 1:
                post(hd, qt, idx)

        pending = []

        def post(hd, qt, idx):
            q0 = qt * 512
            r0, r0b = f32t[0]
            r1, r1b = f32t[1]
            a0, a0b = f32t[2]
            a1, a1b = f32t[3]
            S.op("act", lambda: nc.scalar.copy(out=r0[:], in_=L[0][0][:]), reads=[L[0][1]], writes=[r0b])
            S.op("dve", lambda: nc.vector.tensor_copy(out=r1[:], in_=L[1][0][:]), reads=[L[1][1]], writes=[r1b])
            S.op("act", lambda: nc.scalar.copy(out=a0[:], in_=O[0][0][:]), reads=[O[0][1]], writes=[a0b])
            S.op("dve", lambda: nc.vector.tensor_copy(out=a1[:], in_=O[1][0][:]), reads=[O[1][1]], writes=[a1b])

            def c1():
                S.op("act", lambda: nc.scalar.activation(out=r0[:], in_=r0[:], func=AF.Ln), reads=[r0b], writes=[r0b])
                S.op("act", lambda: nc.scalar.activation(out=r0[:], in_=r0[:], func=AF.Exp, scale=-1.0), reads=[r0b], writes=[r0b])
                S.op("act", lambda: nc.scalar.activation(out=r1[:], in_=r1[:], func=AF.Ln), reads=[r1b], writes=[r1b])
                S.op("act", lambda: nc.scalar.activation(out=r1[:], in_=r1[:], func=AF.Exp, scale=-1.0), reads=[r1b], writes=[r1b])

            def c2():
                S.op("dve", lambda: nc.vector.tensor_tensor(out=a0[:], in0=a0[:], in1=r0[:], op=ALU.mult), reads=[a0b, r0b], writes=[a0b])
                S.op("dve", lambda: nc.vector.scalar_tensor_tensor(out=a1[:], in0=a1[:], scalar=self.lv[:, 0:1], in1=r1[:], op0=ALU.mult, op1=ALU.mult),
                     reads=[a1b, r1b, self.lvb], writes=[a1b])
                S.op("dve", lambda: nc.vector.tensor_tensor(out=a0[:], in0=a0[:], in1=a1[:], op=ALU.subtract), reads=[a0b, a1b], writes=[a0b])

            sq_t, sq_b = sq[0]
            bank = {}

            def c3():
                S.op("act", lambda: nc.scalar.activation(out=sq_t[:], in_=a0[:], func=AF.Square), reads=[a0b], writes=[sq_b])

            def c4():
                bi = spool[self.post_rr % 4]
                self.post_rr += 1
                bank["t"], bank["b"] = self.ps[bi], self.psb[bi]
                S.mm(bank["t"][:], [(self.ones[:], sq_t[:])], bank["b"], [self.onesb, sq_b])

            def c5():
                S.op("act", lambda: nc.scalar.activation(out=r0[:], in_=bank["t"][:], func=AF.Ln, bias=self.eps[:], scale=1.0 / 128),
                     reads=[bank["b"], self.epsb], writes=[r0b])
                S.op("act", lambda: nc.scalar.activation(out=r0[:], in_=r0[:], func=AF.Exp, scale=-0.5), reads=[r0b], writes=[r0b])

            def c6():
                S.op("dve", lambda: nc.vector.scalar_tensor_tensor(out=yd[:, hd, q0:q0 + 512], in0=a0[:], scalar=self.lv[:, 3:4], in1=r0[:],
                                                                   op0=ALU.mult, op1=ALU.mult), reads=[a0b, r0b, self.lvb], writes=[ydb])

            pending.extend([(idx + 1, c1), (idx + 2, c2), (idx + 2, c3), (idx + 3, c4), (idx + 3, c5), (idx + 4, c6)])

        self.post_rr = 0
        for idx in range(len(items) + 1):
            if idx < len(items):
                stage_a(idx)
            if idx >= 1:
                stage_b(idx - 1)
            while pending and pending[0][0] <= idx:
                pending.pop(0)[1]()
        while pending:
            pending.pop(0)[1]()
        self.fence(allb)

    def _zmm(self, W, wb, xnT, xnTb, j, tt):
        A, Ab = self.bank()
        self.S.mm(A[:], [(W[:, kc, j * 128:(j + 1) * 128], xnT[:, kc, tt * 512:(tt + 1) * 512]) for kc in range(8)], Ab, [wb, xnTb])
        return A, Ab

    def p3_sgu(self, st, l, hf, xnT, xnTb, yabc, yabcb, wt, wb):
        nc, S, I = self.nc, self.S, self.I
        tmp = [self.sb(st, f"p3t{i}", [128, 512], F32) for i in range(3)]
        uT, uTb = self.sb(st, "uT", [128, 2, HALF], F32)
        vg, vgb = self.sb(st, "vg", [128, 8, 256], F32)
        st6, st6b = self.sb(st, "st6", [128, 8, 6], F32)
        mv, mvb = self.sb(st, "mv", [128, 8, 2], F32)
        rv, rvb = self.sb(st, "rv", [128, 8], F32)
        vn, vnb = self.sb(st, "vn", [128, 8, 256], BF16)
        bb, bbb = self.sb(st, "bb", [128, 2, 512], F32)
        allb = [uTb, vgb, st6b, mvb, rvb, vnb, bbb] + [b for _, b in tmp]
        self.fresh(allb)
        S.dma("sp", bb[:], I["bb"][l], None, writes=[bbb])
        W = wt[:, 0:12288].rearrange("p (k n) -> p k n", k=8)
        ti = 0
        for tb in range(8):
            A, Ab = self.bank()
            S.mm(A[:, 0:256], [(xnT[:, kc, tb * 128:(tb + 1) * 128], W[:, kc, 1280:1536]) for kc in range(8)], Ab, [wb, xnTb])
            S.op("act", lambda: nc.scalar.activation(out=vg[:, tb, :], in_=A[:, 0:256], func=AF.Gelu), reads=[Ab], writes=[vgb])
            S.op("dve", lambda: nc.vector.bn_stats(out=st6[:, tb, :], in_=vg[:, tb, :]), reads=[vgb], writes=[st6b])
            S.op("dve", lambda: nc.vector.bn_aggr(out=mv[:, tb, :], in_=st6[:, tb, :]), reads=[st6b], writes=[mvb])
        for cc in range(2):
            for tt in range(2):
                A, Ab = self._zmm(W, wb, xnT, xnTb, 8 + cc, tt)
                S.op("act", lambda: nc.scalar.activation(out=uT[:, cc, tt * 512:(tt + 1) * 512], in_=A[:], func=AF.Gelu), reads=[Ab], writes=[uTb])
        S.op("act", lambda: nc.scalar.activation(out=rv[:], in_=mv[:, :, 1], func=AF.Ln, bias=self.eps[:]), reads=[mvb, self.epsb], writes=[rvb])
        S.op("act", lambda: nc.scalar.activation(out=rv[:], in_=rv[:], func=AF.Exp, scale=-0.5), reads=[rvb], writes=[rvb])
        for tb in range(8):
            t_t, t_b = tmp[ti % 3]
            ti += 1
            S.op("dve", lambda: nc.vector.tensor_scalar(out=t_t[:, 0:256], in0=vg[:, tb, :], scalar1=mv[:, tb, 0:1], scalar2=rv[:, tb:tb + 1],
                                                        op0=ALU.subtract, op1=ALU.mult), reads=[vgb, mvb, rvb], writes=[t_b])
            S.op("dve", lambda: nc.vector.tensor_tensor(out=vn[:, tb, :], in0=t_t[:, 0:256], in1=self.lngbc[:], op=ALU.mult),
                 reads=[t_b, self.lngbcb], writes=[vnb])
        for cc in range(2):
            for g4 in range(2):
                A, Ab = self.bank()
                B, Bb = self.bank()
                S.pe_multi([(lambda tbi=tbi: nc.tensor.matmul(A[:, tbi * 128:(tbi + 1) * 128], vn[:, g4 * 4 + tbi, cc * 128:(cc + 1) * 128],
                                                             self.wst[:, 2 * cc, :], start=True, stop=True)) for tbi in range(4)], Ab, [vnb, self.wstb])
                S.pe_multi([(lambda tbi=tbi: nc.tensor.matmul(B[:, tbi * 128:(tbi + 1) * 128], vn[:, g4 * 4 + tbi, cc * 128:(cc + 1) * 128],
                                                             self.wst[:, 2 * cc + 1, :], start=True, stop=True)) for tbi in range(4)], Bb, [vnb, self.wstb])
                t_t, t_b = tmp[ti % 3]
                ti += 1
                S.op("dve", lambda: nc.vector.tensor_tensor(out=t_t[0:64, :], in0=A[0:64, :], in1=bb[0:64, cc, :], op=ALU.add),
                     reads=[Ab, bbb], writes=[t_b])
                S.op("dve", lambda: nc.vector.tensor_tensor(out=t_t[64:128, :], in0=B[64:128, :], in1=bb[64:128, cc, :], op=ALU.add),
                     reads=[Bb, bbb], writes=[t_b])
                S.op("dve", lambda: nc.vector.tensor_tensor(out=yabc[:, 4 + cc, g4 * 512:(g4 + 1) * 512], in0=t_t[:], in1=uT[:, cc, g4 * 512:(g4 + 1) * 512], op=ALU.mult),
                     reads=[t_b, uTb], writes=[yabcb])
        self.fence(allb)

    def p3_poolconv(self, st, l, hf, xnT, xnTb, yabc, yabcb, wt, wb):
        nc, S = self.nc, self.S
        abuf, abufb = self.sb(st, "abuf", [128, 2, 15 + HALF], F32)
        sA, sAb = self.sb(st, "sA", [128, 15 + HALF], F32)
        sB, sBb = self.sb(st, "sB", [128, 15 + HALF], F32)
        pooled, pooledb = self.sb(st, "pooled", [128, 2, HALF], BF16)
        zc, zcb = self.sb(st, "zc", [128, 2, 2 + HALF], F32)
        tmp = [self.sb(st, f"p3u{i}", [128, 512], F32) for i in range(3)]
        allb = [abufb, sAb, sBb, pooledb, zcb] + [b for _, b in tmp]
        self.fresh(allb)
        W = wt[:, 0:12288].rearrange("p (k n) -> p k n", k=8)
        zmm = lambda j, tt: self._zmm(W, wb, xnT, xnTb, j, tt)
        if hf == 0:
            S.op("dve", lambda: nc.vector.memset(abuf[:, :, 0:15], 0.0), writes=[abufb])
            S.op("dve", lambda: nc.vector.memset(zc[:, :, 0:2], 0.0), writes=[zcb])
        else:
            S.op("dve", lambda: nc.vector.tensor_copy(out=abuf[:, :, 0:15], in_=self.hist_a[:]), reads=[self.hist_ab], writes=[abufb])
            S.op("dve", lambda: nc.vector.tensor_copy(out=zc[:, :, 0:2], in_=self.hist_z[:]), reads=[self.hist_zb], writes=[zcb])
        for cc in range(2):
            for tt in range(2):
                A, Ab = zmm(cc, tt)
                S.op("act", lambda: nc.scalar.copy(out=abuf[:, cc, 15 + tt * 512:15 + (tt + 1) * 512], in_=A[:]), reads=[Ab], writes=[abufb])
        ti = 0
        for cc in range(2):
            for tt in range(2):
                Cc, Ccb = zmm(4 + cc, tt)
                Hh, Hhb = zmm(6 + cc, tt)
                t_t, t_b = tmp[ti % 3]
                ti += 1
                S.op("act", lambda: nc.scalar.copy(out=t_t[:], in_=Cc[:]), reads=[Ccb], writes=[t_b])
                S.op("dve", lambda: nc.vector.tensor_tensor(out=zc[:, cc, 2 + tt * 512:2 + (tt + 1) * 512], in0=t_t[:], in1=Hh[:], op=ALU.mult),
                     reads=[t_b, Hhb], writes=[zcb])
        for cc in range(2):
            for tt in range(2):
                Bg, Bgb = zmm(2 + cc, tt)
                y_t, y_b = tmp[ti % 3]
                ti += 1
                a = 2 + tt * 512
                S.op("dve", lambda: nc.vector.tensor_scalar(out=y_t[:], in0=zc[:, cc, a:a + 512], scalar1=self.pv[:, 6 + cc:7 + cc], scalar2=None, op0=ALU.mult),
                     reads=[zcb, self.pvb], writes=[y_b])
                S.op("dve", lambda: nc.vector.scalar_tensor_tensor(out=y_t[:], in0=zc[:, cc, a - 1:a + 511], scalar=self.pv[:, 4 + cc:5 + cc], in1=y_t[:],
                                                                   op0=ALU.mult, op1=ALU.add), reads=[zcb, self.pvb, y_b], writes=[y_b])
                S.op("dve", lambda: nc.vector.scalar_tensor_tensor(out=y_t[:], in0=zc[:, cc, a - 2:a + 510], scalar=self.pv[:, 2 + cc:3 + cc], in1=y_t[:],
                                                                   op0=ALU.mult, op1=ALU.add), reads=[zcb, self.pvb, y_b], writes=[y_b])
                S.op("dve", lambda: nc.vector.tensor_tensor(out=yabc[:, 2 + cc, tt * 512:(tt + 1) * 512], in0=y_t[:], in1=Bg[:], op=ALU.mult),
                     reads=[y_b, Bgb], writes=[yabcb])
        NP_ = 15 + HALF
        for cc in range(2):
            a_ = abuf[:, cc, :]
            S.op("dve", lambda: nc.vector.tensor_tensor(out=sA[:, 1:NP_], in0=a_[:, 1:NP_], in1=a_[:, 0:NP_ - 1], op=ALU.add), reads=[abufb], writes=[sAb])
            if cc == 0:
                S.op("dve", lambda: nc.vector.tensor_tensor(out=sB[64:128, 3:NP_], in0=sA[64:128, 3:NP_], in1=sA[64:128, 1:NP_ - 2], op=ALU.add), reads=[sAb], writes=[sBb])
            else:
                S.op("dve", lambda: nc.vector.tensor_tensor(out=sB[:, 3:NP_], in0=sA[:, 3:NP_], in1=sA[:, 1:NP_ - 2], op=ALU.add), reads=[sAb], writes=[sBb])
                S.op("dve", lambda: nc.vector.tensor_tensor(out=sA[:, 7:NP_], in0=sB[:, 7:NP_], in1=sB[:, 3:NP_ - 4], op=ALU.add), reads=[sBb], writes=[sAb])
                S.op("dve", lambda: nc.vector.tensor_tensor(out=sB[64:128, 15:NP_], in0=sA[64:128, 15:NP_], in1=sA[64:128, 7:NP_ - 8], op=ALU.add), reads=[sAb], writes=[sBb])
            if hf == 0:
                S.op("dve", lambda: nc.vector.tensor_tensor(out=sA[0:64, 15:31], in0=sA[0:64, 15:31], in1=self.corr[0:64, cc, :], op=ALU.mult),
                     reads=[sAb, self.corrb], writes=[sAb])
                S.op("dve", lambda: nc.vector.tensor_tensor(out=sB[64:128, 15:31], in0=sB[64:128, 15:31], in1=self.corr[64:128, cc, :], op=ALU.mult),
                     reads=[sBb, self.corrb], writes=[sBb])
            S.op("dve", lambda: nc.vector.scalar_tensor_tensor(out=pooled[0:64, cc, :], in0=sA[0:64, 15:NP_], scalar=self.pv[0:64, 11 + cc:12 + cc],
                                                               in1=a_[0:64, 15:NP_], op0=ALU.mult, op1=ALU.subtract), reads=[sAb, self.pvb, abufb], writes=[pooledb])
            S.op("dve", lambda: nc.vector.scalar_tensor_tensor(out=pooled[64:128, cc, :], in0=sB[64:128, 15:NP_], scalar=self.pv[64:128, 11 + cc:12 + cc],
                                                               in1=a_[64:128, 15:NP_], op0=ALU.mult, op1=ALU.subtract), reads=[sBb, self.pvb, abufb], writes=[pooledb])
        for cc in range(2):
            for tt in range(2):
                A, Ab = self.bank()
                S.mm(A[:], [(self.pwbd[:, cc, :], pooled[:, cc, tt * 512:(tt + 1) * 512])], Ab, [self.pwbdb, pooledb])
                S.op("act", lambda: nc.scalar.activation(out=yabc[:, cc, tt * 512:(tt + 1) * 512], in_=A[:], func=AF.Copy, scale=self.pv[:, cc:cc + 1]),
                     reads=[Ab, self.pvb], writes=[yabcb])
        if hf == 0:
            S.op("dve", lambda: nc.vector.tensor_copy(out=self.hist_a[:], in_=abuf[:, :, HALF:HALF + 15]), reads=[abufb], writes=[self.hist_ab])
            S.op("dve", lambda: nc.vector.tensor_copy(out=self.hist_z[:], in_=zc[:, :, HALF:HALF + 2]), reads=[zcb], writes=[self.hist_zb])
        self.fence(allb)

    def p45(self, st, l, hf, xnT, xnTb, yabc, yabcb, yd, ydb):
        nc, S = self.nc, self.S
        mT, mTb = self.sb(st, "mT", [128, 8, HALF], BF16)
        G = [self.sb(st, f"G{i}", [128, 512], BF16) for i in range(8)]
        mt = [self.sb(st, f"mt{i}", [128, 512], F32) for i in range(8)]
        allb = [mTb] + [b for _, b in G] + [b for _, b in mt]
        self.fresh(allb)
        gi = 0
        for cp in range(4):
            wt, wb = self.wacquire("p4")
            Wg = wt[:, 0:8192].rearrange("p (i k n) -> p i k n", i=4, k=8)
            Wb = wt[:, 8192:10752].rearrange("p (k n) -> p k n", k=10)
            for ci in range(2):
                c = 2 * cp + ci
                for tt in range(2):
                    ts = slice(tt * 512, (tt + 1) * 512)
                    prods = []
                    for i in range(4):
                        A, Ab = self.bank()
                        S.mm(A[:], [(Wg[:, i, kc, ci * 128:(ci + 1) * 128], xnT[:, kc, ts]) for kc in range(8)], Ab, [wb, xnTb])
                        g_t, g_b = G[gi % 8]
                        S.op("act", lambda: nc.scalar.activation(out=g_t[:], in_=A[:], func=AF.Sigmoid), reads=[Ab], writes=[g_b])
                        P_, Pb = self.bank()
                        if i < 3:
                            prs = [(Wb[:, 2 * i + kk, ci * 128:(ci + 1) * 128], yabc[:, 2 * i + kk, ts]) for kk in range(2)]
                            rd = [wb, yabcb]
                        else:
                            prs = [(Wb[:, 6 + kk, ci * 128:(ci + 1) * 128], yd[:, kk, ts]) for kk in range(4)]
                            rd = [wb, ydb]
                        S.mm(P_[:], prs, Pb, rd)
                        m_t, m_b = mt[gi % 8]
                        gi += 1
                        S.op("dve", lambda: nc.vector.tensor_tensor(out=m_t[:], in0=g_t[:], in1=P_[:], op=ALU.mult), reads=[g_b, Pb], writes=[m_b])
                        prods.append((m_t, m_b))
                    (m0, b0), (m1, b1), (m2, b2), (m3, b3) = prods
                    S.op("dve", lambda: nc.vector.tensor_tensor(out=m0[:], in0=m0[:], in1=m1[:], op=ALU.add), reads=[b0, b1], writes=[b0])
                    S.op("dve", lambda: nc.vector.tensor_tensor(out=m2[:], in0=m2[:], in1=m3[:], op=ALU.add), reads=[b2, b3], writes=[b2])
                    S.op("dve", lambda: nc.vector.tensor_tensor(out=mT[:, c, ts], in0=m0[:], in1=m2[:], op=ALU.add), reads=[b0, b2], writes=[mTb])
        wt, wb = self.wacquire("wout")
        Wo = wt[:, 0:8192].rearrange("p (k n) -> p k n", k=8)
        for tb in range(8):
            for h2 in range(2):
                A, Ab = self.bank()
                S.mm(A[:], [(mT[:, kc, tb * 128:(tb + 1) * 128], Wo[:, kc, h2 * 512:(h2 + 1) * 512]) for kc in range(8)], Ab, [wb, mTb])
                hs = self.h[:, hf * 8 + tb, h2 * 512:(h2 + 1) * 512]
                hqb = self.hq[(hf * 8 + tb) // 4]
                S.op("dve", lambda: nc.vector.tensor_tensor(out=hs, in0=A[:], in1=hs, op=ALU.add), reads=[Ab, hqb], writes=[hqb])
        self.fence(allb)

    def ffn_phase(self, l):
        nc, S, I = self.nc, self.S, self.I
        moe = (l % 2 == 1)
        with ExitStack() as st:
            hnT, hnTb = self.sb(st, "hnT", [128, 8, SEQ], BF16)
            comb, combb = self.sb(st, "comb", [128, 16, NE], F32)
            self.gbc2, self.gbc2b = self.sb(st, "gbc2", [128, D], F32)
            allb = [hnTb, combb, self.gbc2b]
            self.fresh(allb)
            S.dma("sp", self.gbc2[:], I["norm2_g"][l].partition_broadcast(128), None, writes=[self.gbc2b])
            ab = [self.sb(st, "actT0", [128, 4, SEQ], BF16)]
            sg = [self.sb(st, f"sg{i}", [128, 512], F32) for i in range(3)]
            ab_sg = [b for _, b in ab] + [b for _, b in sg]
            allb += ab_sg
            self.fresh(ab_sg)
            router = None
            rs_ = ExitStack()
            if moe:
                rwf, rwfb = self.sb(rs_, "rwf", [128, 8, NE], F32)
                rwh, rwhb = self.sb(rs_, "rwh", [128, 8, NE], BF16)
                rwl, rwlb = self.sb(rs_, "rwl", [128, 8, NE], BF16)
                xf, xfb = self.sb(rs_, "xf", [128, D], F32)
                xl, xlb = self.sb(rs_, "xl", [128, D], BF16)
                xlT, xlTb = self.sb(rs_, "xlT", [128, 8, 128], BF16)
                lg, lgb = self.sb(rs_, "lg", [128, 16, NE], F32)
                mx, mxb = self.sb(rs_, "mx", [128, 16, 8], F32)
                wv_, wvb = self.sb(rs_, "wv", [128, 16, 4], F32)
                rb = [rwfb, rwhb, rwlb, xfb, xlb, xlTb, lgb, mxb, wvb]
                self.fresh(rb)
                S.dma("sp", rwf[:], I["router_w"][0].rearrange("(kc p) e -> p kc e", p=128), None, writes=[rwfb])
                S.op("dve", lambda: nc.vector.tensor_copy(out=rwh[:], in_=rwf[:]), reads=[rwfb], writes=[rwhb])
                S.op("dve", lambda: nc.vector.tensor_tensor(out=rwl[:], in0=rwf[:], in1=rwh[:], op=ALU.subtract), reads=[rwfb, rwhb], writes=[rwlb])

                lg_pending = []

                def router(i, x_t, x_b, rs, rsb):
                    flush_lg()
                    S.op("dve", lambda: nc.vector.scalar_tensor_tensor(out=xf[:], in0=self.h[:, i, :], scalar=rs[:, i:i + 1], in1=self.gbc2[:],
                                                                       op0=ALU.mult, op1=ALU.mult), reads=[self.hq[i // 4], rsb, self.gbc2b], writes=[xfb])
                    S.op("dve", lambda: nc.vector.tensor_tensor(out=xl[:], in0=xf[:], in1=x_t[:], op=ALU.subtract), reads=[xfb, x_b], writes=[xlb])
                    pt, pb = self.bank()
                    ptv = pt[:].bitcast(BF16)
                    S.pe_multi([(lambda c=c: nc.tensor.transpose(out=ptv[:, c * 128:(c + 1) * 128], in_=xl[:, c * 128:(c + 1) * 128], identity=self.ident[:]))
                                for c in range(8)], pb, [xlb, self.identb])
                    S.op("act", lambda: nc.scalar.copy(out=xlT[:], in_=ptv.rearrange("p (c t) -> p c t", c=8)), reads=[pb], writes=[xlTb])
                    L_, Lb = self.bank()
                    prs = []
                    for kc in range(8):
                        hi = hnT[:, kc, i * 128:(i + 1) * 128]
                        prs += [(hi, rwh[:, kc, :]), (hi, rwl[:, kc, :]), (xlT[:, kc, :], rwh[:, kc, :])]
                    S.mm(L_[:, 0:NE], prs, Lb, [hnTb, xlTb, rwhb, rwlb])
                    lg_pending.append((i, L_, Lb))

                def flush_lg():
                    while lg_pending:
                        i_, L__, Lb_ = lg_pending.pop(0)
                        S.op("dve", lambda: nc.vector.tensor_copy(out=lg[:, i_, :], in_=L__[:, 0:NE]), reads=[Lb_], writes=[lgb])

                def router_finish():
                    flush_lg()
                    for i in range(16):
                        S.op("dve", lambda: nc.vector.max(out=mx[:, i, :], in_=lg[:, i, :]), reads=[lgb], writes=[mxb])
                    S.op("dve", lambda: nc.vector.tensor_tensor(out=wv_[:, :, 0], in0=mx[:, :, 1], in1=mx[:, :, 0], op=ALU.subtract), reads=[mxb], writes=[wvb])
                    S.op("act", lambda: nc.scalar.activation(out=wv_[:, :, 1], in_=wv_[:, :, 0], func=AF.Sigmoid, scale=-1.0), reads=[wvb], writes=[wvb])
                    S.op("act", lambda: nc.scalar.activation(out=wv_[:, :, 2], in_=wv_[:, :, 0], func=AF.Sigmoid), reads=[wvb], writes=[wvb])
                    for i in range(16):
                        S.op("dve", lambda: nc.vector.tensor_scalar(out=comb[:, i, :], in0=lg[:, i, :], scalar1=mx[:, i, 0:1], scalar2=wv_[:, i, 1:2],
                                                                    op0=ALU.is_equal, op1=ALU.mult), reads=[lgb, mxb, wvb], writes=[combb])
                        S.op("dve", lambda: nc.vector.tensor_scalar(out=xf[:, i * NE:(i + 1) * NE], in0=lg[:, i, :], scalar1=mx[:, i, 1:2], scalar2=wv_[:, i, 2:3],
                                                                    op0=ALU.is_equal, op1=ALU.mult), reads=[lgb, mxb, wvb], writes=[xfb])
                    S.op("dve", lambda: nc.vector.tensor_tensor(out=comb[:], in0=comb[:], in1=xf[:, 0:16 * NE].rearrange("p (a b) -> p a b", a=16), op=ALU.add),
                         reads=[combb, xfb], writes=[combb])

            groups = self.ffn_groups(l)
            self.k3 = 0

            def views(wt, nch):
                Wgt = wt[:, 0:8 * nch * 128].rearrange("p (k n) -> p k n", k=8)
                Wup = wt[:, 4096:4096 + 8 * nch * 128].rearrange("p (k n) -> p k n", k=8)
                Wdn = wt[:, 8192:8192 + nch * 1024].rearrange("p (c n) -> p c n", c=nch)
                return Wgt, Wup, Wdn

            def emit_up(wt, wb, nch, a_t, a_b, tts):
                Wgt, Wup, _ = views(wt, nch)
                for tt in tts:
                    ts = slice(tt * 512, (tt + 1) * 512)
                    for j in range(nch):
                        Gp, Gb = self.bank()
                        S.mm(Gp[:], [(Wgt[:, kc, j * 128:(j + 1) * 128], hnT[:, kc, ts]) for kc in range(8)], Gb, [wb, hnTb])
                        Up, Ub = self.bank()
                        S.mm(Up[:], [(Wup[:, kc, j * 128:(j + 1) * 128], hnT[:, kc, ts]) for kc in range(8)], Ub, [wb, hnTb])
                        s_t, s_b = sg[self.k3 % 3]
                        self.k3 += 1
                        S.op("act", lambda: nc.scalar.activation(out=s_t[:], in_=Gp[:], func=AF.Silu), reads=[Gb], writes=[s_b])
                        S.op("dve", lambda: nc.vector.tensor_tensor(out=a_t[:, j, ts], in0=s_t[:], in1=Up[:], op=ALU.mult), reads=[s_b, Ub], writes=[a_b])

            def emit_down(wt, wb, e, nch, a_t, a_b, last=None):
                _, _, Wdn = views(wt, nch)
                for tb in range(16):
                    for h2 in range(2):
                        Op, Ob = self.bank()
                        S.mm(Op[:], [(a_t[:, j, tb * 128:(tb + 1) * 128], Wdn[:, j, h2 * 512:(h2 + 1) * 512]) for j in range(nch)], Ob, [wb, a_b])
                        hs = self.h[:, tb, h2 * 512:(h2 + 1) * 512]
                        sc = 1.0 if e is None else comb[:, tb, e:e + 1]
                        hqb = self.hq[tb // 4]
                        rd = [Ob, hqb] + ([] if e is None else [combb])
                        S.op("dve", lambda: nc.vector.scalar_tensor_tensor(out=hs, in0=Op[:], scalar=sc, in1=hs, op0=ALU.mult, op1=ALU.add),
                             reads=rd, writes=[hqb])
                    if last and tb % 4 == 3:
                        last(tb // 4)

            e0, j00, nch0 = groups[0]
            wt0, wb0 = self.wacquire("ffn")

            def after_block(i):
                if i % 4 == 3:
                    if moe:
                        flush_lg()
                    emit_up(wt0, wb0, nch0, ab[0][0], ab[0][1], [i // 4])

            with ExitStack() as s1:
                self.norm_phase(s1, 0, 16, self.gbc2, self.gbc2b, hnT, hnTb, router=router, after_block=after_block)
            if moe:
                router_finish()
                self.fence(rb)
            rs_.close()
            ab.append(self.sb(st, "actT1", [128, 4, SEQ], BF16))
            allb.append(ab[1][1])
            self.fresh([ab[1][1]])
            for gidx, (e, j0, nch) in enumerate(groups):
                a_t, a_b = ab[gidx % 2]
                if gidx == 0:
                    wt, wb = wt0, wb0
                else:
                    wt, wb = self.wacquire("ffn")
                    emit_up(wt, wb, nch, a_t, a_b, range(4))
                emit_down(wt, wb, e, nch, a_t, a_b, last=(self.store_cb if gidx == len(groups) - 1 else None))
            self.fence(allb)


def _build(cfg=None):
    p = Prog(cfg)
    p.released = []
    nc = p.build()
    return nc, p


def _rel_bucket_table():
    import jax
    import jax.numpy as jnp
    with jax.default_device(jax.devices("cpu")[0]):
        rel = jnp.arange(-700, 200)
        nb, max_exact = 16, 8
        n = jnp.abs(rel)
        nf = jnp.maximum(n, 1).astype(jnp.float32)
        large = max_exact + (jnp.log(nf / max_exact) / math.log(128 / max_exact) * (nb - max_exact)).astype(jnp.int32)
        large = jnp.minimum(large, nb - 1)
        b = jnp.where(rel > 0, nb, 0) + jnp.where(n < max_exact, n, large)
        return np.asarray(b), 700


def _host_layout(inp):
    f = lambda a: np.ascontiguousarray(np.asarray(a, dtype=np.float32))
    c = {}
    for k in ("w_in", "w_branch_pool", "w_branch_conv", "w_branch_sgu", "w_branch_attn", "w_out", "ffn_w_gate_up",
              "ffn_w_down", "router_w", "moe_w_gate_up", "moe_w_down", "norm1_g", "norm2_g", "sgu_ln_g"):
        c[k] = f(inp[k])
    c["diff_lambda"] = f(inp["diff_lambda"]).reshape(2, 256)
    pv = np.zeros((2, 128, NPV), np.float32)
    for l in range(2):
        pv[l, :, 0:2] = f(inp["pool_scale"])[l].reshape(2, 128).T
        cw = f(inp["conv_w"])[l]
        for tap in range(3):
            pv[l, :, 2 + 2 * tap:4 + 2 * tap] = cw[tap].reshape(2, 128).T
        pv[l, :, 8] = np.tile(f(inp["q_norm_g"])[l], 2)
        pv[l, :, 9] = np.tile(f(inp["k_norm_g"])[l], 2)
        pv[l, :, 10] = f(inp["subln_g"])[l]
        pv[l, 0:64, 11], pv[l, 64:128, 11] = 1.0 / 2, 1.0 / 4
        pv[l, 0:64, 12], pv[l, 64:128, 12] = 1.0 / 8, 1.0 / 16
    c["pvec"] = pv
    sb_ = f(inp["sgu_b"])
    bb = np.zeros((2, 128, 2, 512), np.float32)
    for cc in range(2):
        for hh in range(2):
            bb[:, hh * 64:(hh + 1) * 64, cc, :] = np.tile(sb_[:, 2 * cc + hh, :], (1, 4))[:, None, :]
    c["bb"] = bb
    c["wst"] = np.ascontiguousarray(f(inp["sgu_w"]).transpose(0, 3, 1, 2))
    pw = f(inp["pool_w"])
    pwbd = np.zeros((2, 128, 2, 128), np.float32)
    for g in range(4):
        r = (g % 2) * 64
        pwbd[:, r:r + 64, g // 2, r:r + 64] = pw[:, g]
    c["pwbd"] = pwbd
    tab, off = _rel_bucket_table()
    kk = np.arange(128)[:, None]
    jj = np.arange(640)[None, :]
    bidx = tab[(kk - jj) + off]
    rb = f(inp["rel_bias"])
    strips = np.ascontiguousarray(rb[bidx].transpose(0, 2, 1))
    masked = np.broadcast_to(((jj < 128) & ((jj // 64) < (kk // 64)))[:, None, :], strips.shape)
    c["strips"] = np.ascontiguousarray(np.where(masked, np.float32(NEG), strips))
    c["c15"] = np.ascontiguousarray(np.broadcast_to(rb[15][None, :], (128, 8)))
    c["ident"] = np.eye(128, dtype=np.float32)
    c["trimask"] = (np.arange(128)[None, :] >= np.arange(128)[:, None]).astype(np.float32)
    corr = np.ones((128, 2, 16), np.float32)
    wins = {(0, 0): 2, (0, 1): 4, (1, 0): 8, (1, 1): 16}
    for (cc, hh), w in wins.items():
        t = np.arange(16)
        corr[hh * 64:(hh + 1) * 64, cc, :] = (w / np.minimum(t + 1, w))[None, :]
    c["corr"] = corr
    bo = np.zeros((128, 128), np.float32)
    bo[0:64, 0:64] = 1.0
    bo[64:128, 64:128] = 1.0
    c["blockones"] = bo
    return c


_CACHE = {}


def kernel(**inputs):
    common = _host_layout(inputs)
    x = np.ascontiguousarray(np.asarray(inputs["x"], dtype=np.float32))
    if "nc" not in _CACHE:
        _CACHE["nc"] = _build()[0]
    nc = _CACHE["nc"]
    in_maps = []
    for i in range(N_CORES):
        m = dict(common)
        m["x"] = x[2 * i:2 * i + 2]
        in_maps.append(m)
    res = run_bass_kernel_spmd(nc, in_maps, core_ids=list(range(N_CORES)))
    return np.concatenate([r["out"] for r in res.results], axis=0).astype(np.float32)
```
